# Optimizing a Trainium2 kernel written in Bass

```python
import jax, jax.numpy as jnp
from jax import lax
import numpy as np

D_MODEL = 1024
BATCH = 8
SEQ = 4096
DEPTH = 1

HEAD_DIM = 64
N_HEADS = (D_MODEL // 2) // HEAD_DIM
N_KV_HEADS = 2
GROUP = N_HEADS // N_KV_HEADS
Q_COLS = N_HEADS * HEAD_DIM
KV_COLS = N_KV_HEADS * HEAD_DIM
WINDOW = 128
ATTN_BLOCK = 128
NUM_BUCKETS = 32
MAX_DISTANCE = 128
CONV_CH = D_MODEL - Q_COLS
CONV_WIDTH = 31
IN_COLS = Q_COLS + 2 * KV_COLS + 2 * CONV_CH
N_EXPERTS = 256
TOP_K = 8
N_GROUPS = 8
TOPK_GROUPS = 4
EXPERT_HIDDEN = 256
SHARED_HIDDEN = 256
ROUTED_SCALE = 2.5
MOE_BLOCK = 128
EPS = 1e-6

kernel_name = "hybrid_conv_swa_sink_t5bias_moe_adaln"


def _rms_norm(x):
    xf = x.astype(jnp.float32)
    return (xf * lax.rsqrt(jnp.mean(xf * xf, axis=-1, keepdims=True) + EPS)).astype(x.dtype)


def _layer_norm(x, g, b):
    xf = x.astype(jnp.float32)
    mu = jnp.mean(xf, axis=-1, keepdims=True)
    var = jnp.mean(jnp.square(xf - mu), axis=-1, keepdims=True)
    y = (xf - mu) * lax.rsqrt(var + EPS) * g.astype(jnp.float32) + b.astype(jnp.float32)
    return y.astype(x.dtype)


def _t5_causal_buckets(dist):
    n = np.maximum(dist, 0)
    max_exact = NUM_BUCKETS // 2
    large = max_exact + (np.log(np.maximum(n, 1) / max_exact) / np.log(MAX_DISTANCE / max_exact)
                         * (NUM_BUCKETS - max_exact)).astype(np.int32)
    large = np.minimum(large, NUM_BUCKETS - 1)
    return np.where(n < max_exact, n, large).astype(np.int32)


def _sliding_window_attention(q, k, v, sinks, rel_bias):
    B, S, _ = q.shape
    L = ATTN_BLOCK
    nb = S // L
    q = q.reshape(B, nb, L, N_KV_HEADS, GROUP, HEAD_DIM)
    k = k.reshape(B, nb, L, N_KV_HEADS, HEAD_DIM)
    v = v.reshape(B, nb, L, N_KV_HEADS, HEAD_DIM)
    pad = ((0, 0), (1, 0), (0, 0), (0, 0), (0, 0))
    kk = jnp.concatenate([jnp.pad(k, pad)[:, :-1], k], axis=2)
    vv = jnp.concatenate([jnp.pad(v, pad)[:, :-1], v], axis=2)
    qi = np.arange(L)[:, None]
    ki = np.arange(2 * L)[None, :]
    dist = qi + L - ki
    band = (dist >= 0) & (dist < WINDOW)
    not_first = np.arange(nb)[:, None, None] > 0
    mask = band[None] & (not_first | (ki >= L)[None])
    bias = rel_bias.astype(jnp.float32)[_t5_causal_buckets(dist)]
    bias = jnp.transpose(bias, (2, 0, 1)).reshape(N_KV_HEADS, GROUP, L, 2 * L)
    logits = jnp.einsum('bnqkgd,bnskd->bnkgqs', q, kk,
                        preferred_element_type=jnp.float32) * (HEAD_DIM ** -0.5) + bias
    logits = jnp.where(jnp.asarray(mask)[None, :, None, None], logits, -jnp.inf)
    sink = sinks.astype(jnp.float32).reshape(1, 1, N_KV_HEADS, GROUP, 1, 1)
    m = jnp.maximum(jnp.max(logits, axis=-1, keepdims=True), sink)
    p = jnp.exp(logits - m)
    denom = jnp.sum(p, axis=-1) + jnp.exp(sink - m)[..., 0]
    o = jnp.einsum('bnkgqs,bnskd->bnqkgd', p, vv.astype(jnp.float32))
    o = o / jnp.transpose(denom, (0, 1, 4, 2, 3))[..., None]
    return o.reshape(B, S, Q_COLS).astype(q.dtype)


def _conv_module(a, gate, conv_w, conv_b, ln_g, ln_b):
    u = a * jax.nn.sigmoid(gate)
    u = lax.conv_general_dilated(u, conv_w[:, None, :], window_strides=(1,),
                                 padding=[(CONV_WIDTH - 1, 0)],
                                 dimension_numbers=('NWC', 'WIO', 'NWC'),
                                 feature_group_count=CONV_CH) + conv_b
    return jax.nn.silu(_layer_norm(u, ln_g, ln_b))


def _moe(h, w_router, router_bias, w_gate, w_up, w_down, w_sh_gate, w_sh_up, w_sh_down):
    B, S, D = h.shape
    T = B * S
    ht = h.reshape(T, D)
    scores = jax.nn.sigmoid(jnp.dot(ht, w_router, preferred_element_type=jnp.float32))
    sel = scores + router_bias.astype(jnp.float32)
    grp = sel.reshape(T, N_GROUPS, N_EXPERTS // N_GROUPS)
    grp_score = jnp.sum(lax.top_k(grp, 2)[0], axis=-1)
    _, gidx = lax.top_k(grp_score, TOPK_GROUPS)
    gmask = jnp.sum(jax.nn.one_hot(gidx, N_GROUPS, dtype=jnp.float32), axis=-2) > 0
    emask = jnp.repeat(gmask, N_EXPERTS // N_GROUPS, axis=-1)
    _, eidx = lax.top_k(jnp.where(emask, sel, -jnp.inf), TOP_K)
    gw = jnp.take_along_axis(scores, eidx, axis=-1)
    gw = gw / jnp.sum(gw, axis=-1, keepdims=True) * ROUTED_SCALE
    A = T * TOP_K
    flat_e = eidx.reshape(A)
    order = jnp.argsort(flat_e)
    se = flat_e[order]
    stok = (order // TOP_K).astype(jnp.int32)
    sw = gw.reshape(A)[order]
    counts = jnp.bincount(flat_e, length=N_EXPERTS)
    starts = jnp.cumsum(counts) - counts
    padded = (counts + MOE_BLOCK - 1) // MOE_BLOCK * MOE_BLOCK
    pends = jnp.cumsum(padded)
    pstarts = pends - padded
    dest = pstarts[se] + jnp.arange(A, dtype=jnp.int32) - starts[se]
    n_blocks = (A + MOE_BLOCK - 1) // MOE_BLOCK + N_EXPERTS
    P = n_blocks * MOE_BLOCK
    row_tok = jnp.full((P,), T, jnp.int32).at[dest].set(stok)
    row_w = jnp.zeros((P,), jnp.float32).at[dest].set(sw)
    blk_e = jnp.minimum(jnp.searchsorted(pends, jnp.arange(n_blocks, dtype=jnp.int32) * MOE_BLOCK,
                                         side='right'), N_EXPERTS - 1)
    h_pad = jnp.concatenate([ht, jnp.zeros((1, D), ht.dtype)], axis=0)

    def step(acc, blk):
        tok, w, e = blk
        xb = h_pad[tok]
        hid = jax.nn.silu(xb @ w_gate[e]) * (xb @ w_up[e])
        out = (hid @ w_down[e]).astype(jnp.float32) * w[:, None]
        return acc.at[tok].add(out), None

    acc, _ = lax.scan(step, jnp.zeros((T + 1, D), jnp.float32),
                      (row_tok.reshape(n_blocks, MOE_BLOCK), row_w.reshape(n_blocks, MOE_BLOCK), blk_e))
    shared = (jax.nn.silu(ht @ w_sh_gate) * (ht @ w_sh_up)) @ w_sh_down
    return (acc[:T] + shared.astype(jnp.float32)).astype(h.dtype).reshape(B, S, D)


def setup_inputs(seed: int = 0) -> dict:
    key = jax.random.key(seed)
    ks = jax.random.split(key, 24)

    def nrm(k, shape, s):
        return jax.random.normal(k, shape, jnp.float32) * s

    D = D_MODEL
    return {
        "x": nrm(ks[0], (BATCH, SEQ, D), 1.0),
        "c": nrm(ks[1], (BATCH, D), 1.0),
        "w_ada": nrm(ks[2], (DEPTH, D, 6 * D), 0.5 * D ** -0.5),
        "b_ada": nrm(ks[3], (DEPTH, 6 * D), 0.02),
        "w_in": nrm(ks[4], (DEPTH, D, IN_COLS), D ** -0.5),
        "attn_sinks": nrm(ks[5], (DEPTH, N_HEADS), 1.0),
        "rel_bias": nrm(ks[6], (NUM_BUCKETS, N_HEADS), 0.5),
        "attn_norm_g": 1.0 + nrm(ks[7], (DEPTH, Q_COLS), 0.02),
        "conv_w": nrm(ks[8], (DEPTH, CONV_WIDTH, CONV_CH), CONV_WIDTH ** -0.5),
        "conv_b": nrm(ks[9], (DEPTH, CONV_CH), 0.02),
        "conv_ln_g": 1.0 + nrm(ks[10], (DEPTH, CONV_CH), 0.02),
        "conv_ln_b": nrm(ks[11], (DEPTH, CONV_CH), 0.02),
        "w_out": nrm(ks[12], (DEPTH, D, D), D ** -0.5),
        "w_router": nrm(ks[13], (DEPTH, D, N_EXPERTS), D ** -0.5),
        "router_bias": nrm(ks[14], (DEPTH, N_EXPERTS), 0.01),
        "w_exp_gate": nrm(ks[15], (DEPTH, N_EXPERTS, D, EXPERT_HIDDEN), D ** -0.5),
        "w_exp_up": nrm(ks[16], (DEPTH, N_EXPERTS, D, EXPERT_HIDDEN), D ** -0.5),
        "w_exp_down": nrm(ks[17], (DEPTH, N_EXPERTS, EXPERT_HIDDEN, D), EXPERT_HIDDEN ** -0.5),
        "w_sh_gate": nrm(ks[18], (DEPTH, D, SHARED_HIDDEN), D ** -0.5),
        "w_sh_up": nrm(ks[19], (DEPTH, D, SHARED_HIDDEN), D ** -0.5),
        "w_sh_down": nrm(ks[20], (DEPTH, SHARED_HIDDEN, D), SHARED_HIDDEN ** -0.5),
        "final_norm_g": 1.0 + nrm(ks[21], (D,), 0.02),
    }


def reference(x, c, w_ada, b_ada, w_in, attn_sinks, rel_bias, attn_norm_g, conv_w, conv_b,
              conv_ln_g, conv_ln_b, w_out, w_router, router_bias, w_exp_gate, w_exp_up,
              w_exp_down, w_sh_gate, w_sh_up, w_sh_down, final_norm_g):
    s_q = Q_COLS
    s_k = s_q + KV_COLS
    s_v = s_k + KV_COLS
    s_a = s_v + CONV_CH
    for l in range(DEPTH):
        mod = jnp.dot(jax.nn.silu(c), w_ada[l]) + b_ada[l]
        sh1, sc1, g1, sh2, sc2, g2 = jnp.split(mod, 6, axis=-1)
        h = _rms_norm(x) * (1 + sc1[:, None]) + sh1[:, None]
        proj = h @ w_in[l]
        q = proj[..., :s_q]
        k = proj[..., s_q:s_k]
        v = proj[..., s_k:s_v]
        a = proj[..., s_v:s_a]
        gt = proj[..., s_a:]
        attn = _rms_norm(_sliding_window_attention(q, k, v, attn_sinks[l], rel_bias)) * attn_norm_g[l]
        conv = _conv_module(a, gt, conv_w[l], conv_b[l], conv_ln_g[l], conv_ln_b[l])
        mixed = jnp.concatenate([attn, conv], axis=-1) @ w_out[l]
        x = x + g1[:, None] * mixed
        h2 = _rms_norm(x) * (1 + sc2[:, None]) + sh2[:, None]
        x = x + g2[:, None] * _moe(h2, w_router[l], router_bias[l], w_exp_gate[l], w_exp_up[l],
                                    w_exp_down[l], w_sh_gate[l], w_sh_up[l], w_sh_down[l])
    return _rms_norm(x) * final_norm_g
```

```python
import contextlib
import os
import numpy as np
import concourse.bass as bass
import concourse.mybir as mybir
from concourse.bass_utils import run_bass_kernel_spmd

F32 = mybir.dt.float32
BF16 = mybir.dt.bfloat16
I32 = mybir.dt.int32
ALU = mybir.AluOpType
AF = mybir.ActivationFunctionType

S = 4096
D = 1024
NT = 8
NB = 32
E = 256
RB = 256
NBLK = S * 8 // RB + E
EPS = 1e-6


class Buf:
    def __init__(self, name):
        self.name = name
        self.writers = {}
        self.readers = {}
        self.dsem = None
        self.dcnt = 0


class Trk:
    def __init__(self, nc, es):
        self.nc = nc
        self.es = es
        self.eng = {"pe": nc.tensor, "act": nc.scalar, "dve": nc.vector, "pool": nc.gpsimd, "sp": nc.sync}
        self.sem = {}
        self.cnt = {}
        self.semobj = {}
        for k in self.eng:
            s = es.enter_context(nc.semaphore("s_" + k))
            self.sem[k] = s
            self.cnt[k] = 0
            self.semobj[("e", k)] = s
        self.waited = {}
        self.nd = 0
        self.log = {k: [] for k in self.eng}
        self.pool_q = []

    def buf(self, name):
        return Buf(name)

    def _wait(self, ename, deps):
        e = self.eng[ename]
        for key, val in deps.items():
            if key == ("e", ename):
                continue
            if self.waited.get((ename, key), 0) >= val:
                continue
            e.wait_ge(self.semobj[key], val)
            self.log[ename].append(("wait", key, val))
            self.waited[(ename, key)] = val

    @staticmethod
    def _merge(d, key, val):
        if d.get(key, 0) < val:
            d[key] = val

    def _deps(self, reads, writes, nowaw):
        deps = {}
        for b in reads:
            for k, v in b.writers.items():
                self._merge(deps, k, v)
        for b in writes:
            if not nowaw:
                for k, v in b.writers.items():
                    self._merge(deps, k, v)
            for k, v in b.readers.items():
                self._merge(deps, k, v)
        return deps

    def _record(self, tok, reads, writes, nowaw):
        key, val = tok
        for b in reads:
            self._merge(b.readers, key, val)
        for b in writes:
            if nowaw:
                self._merge(b.writers, key, val)
            else:
                b.writers = {key: val}
                b.readers = {}

    def op(self, ename, fn, reads=(), writes=(), sig=True, nowaw=False, fence=False):
        self._wait(ename, self._deps(reads, writes, nowaw))
        ins = fn(self.eng[ename])
        if fence:
            ins.then_inc(self.sem[ename], 1)
            self.cnt[ename] += 1
            self.log[ename].append(("inc", ("e", ename), 1))
            tok = (("e", ename), self.cnt[ename])
            self._record(tok, reads, writes, nowaw)
            other = "act" if ename == "dve" else "dve"
            self._wait(other, {("e", ename): self.cnt[ename]})
            fz = self.fence_ap
            if other == "act":
                i2 = self.eng[other].activation(out=fz[:, 0:1], in_=fz[:, 1:2], func=AF.Copy)
            else:
                i2 = self.eng[other].tensor_copy(out=fz[:, 2:3], in_=fz[:, 3:4])
            i2.then_inc(self.sem[other], 1)
            self.cnt[other] += 1
            self.log[other].append(("inc", ("e", other), 1))
            self._wait(ename, {("e", other): self.cnt[other]})
            return ins
        if sig:
            ins.then_inc(self.sem[ename], 1)
            self.cnt[ename] += 1
            self.log[ename].append(("inc", ("e", ename), 1))
            tok = (("e", ename), self.cnt[ename])
        else:
            tok = (("e", ename), self.cnt[ename] + 1)
        self._record(tok, reads, writes, nowaw)
        return ins

    POOL_WINDOW = 4

    def dma(self, qname, fn, reads=(), writes=(), semb=None, nowaw=False):
        self._wait(qname, self._deps(reads, writes, nowaw))
        if qname == "pool":
            while len(self.pool_q) >= self.POOL_WINDOW:
                k0, v0 = self.pool_q.pop(0)
                self._wait("pool", {k0: v0})
        if semb is None:
            semb = writes[0]
        if semb.dsem is None:
            self.nd += 1
            semb.dsem = self.es.enter_context(self.nc.semaphore("d%d" % self.nd))
            self.semobj[("d", id(semb))] = semb.dsem
        ins = fn(self.eng[qname])
        ins.then_inc(semb.dsem, 16)
        self.log[qname].append(("inc", ("d", id(semb)), 16))
        semb.dcnt += 16
        tok = (("d", id(semb)), semb.dcnt)
        if qname == "pool":
            self.pool_q.append(tok)
        self._record(tok, reads, writes, nowaw)
        return ins

    def simulate(self):
        val = {}
        pos = {k: 0 for k in self.log}
        prog = True
        while prog:
            prog = False
            for k, lg in self.log.items():
                while pos[k] < len(lg):
                    kind, key, v = lg[pos[k]]
                    if kind == "wait":
                        if val.get(key, 0) < v:
                            break
                    else:
                        val[key] = val.get(key, 0) + v
                    pos[k] += 1
                    prog = True
        stuck = {k: (pos[k], len(lg), lg[pos[k]]) for k, lg in self.log.items() if pos[k] < len(lg)}
        return stuck, val

    def finish(self, ename, bufs):
        deps = {}
        for b in bufs:
            for k, v in b.writers.items():
                self._merge(deps, k, v)
            for k, v in b.readers.items():
                self._merge(deps, k, v)
        self._wait(ename, deps)


class _StopBuild(Exception):
    pass


def build(stage=99):
    nc = bass.Bass("TRN2", target_bir_lowering=False)
    try:
        _build(nc, stage)
    except _StopBuild:
        pass
    if os.environ.get("MK_SIM"):
        stuck, val = _TRK[0].simulate()
        names = {k: v for k, v in _TRK[0].semobj.items()}
        print("SIM stuck:", stuck)
        for k, (p, n, ev) in stuck.items():
            print(k, p, n, ev, "current", val.get(ev[1], 0))
    return nc


_TRK = [None]


def _build(nc, stage):
    dbg_tag = os.environ.get("MK_DBG", "")

    def din(name, shape, dt=F32):
        return nc.dram_tensor(name, list(shape), dt, kind="ExternalInput").ap()

    x_d = din("x", [S, D])
    cfm_d = din("cfm", [128, 8])
    wada_d = din("w_ada", [D, 6 * D])
    bada_d = din("b_ada", [1, 6 * D])
    win_d = din("w_in", [D, 1792])
    sinkfm_d = din("sinkfm", [128, 4])
    biasg_d = din("biasg", [2, 2, 128, 512])
    maskc_d = din("maskc", [2, 128, 512])
    angfm_d = din("angfm", [128, 4])
    convw_d = din("convw", [128, 4 * 31])
    convb_d = din("convb", [128, 4])
    lng_d = din("lng", [128, 4])
    lnb_d = din("lnb", [128, 4])
    wout_d = din("w_out", [D, D])
    wr_d = din("w_router", [D, E])
    rb_d = din("router_bias", [1, E])
    NEW = int(os.environ.get("MK_SMALLW", E))
    weg_d = din("w_exp_gate", [NEW * 128, 2048])
    weu_d = din("w_exp_up", [NEW * 128, 2048])
    wed_d = din("w_exp_down", [NEW * 128, 2048])
    wsg_d = din("w_sh_gate", [D, 256])
    wsu_d = din("w_sh_up", [D, 256])
    wsd_d = din("w_sh_down", [256, D])
    fng_d = din("final_norm_g", [1, D])
    cst_d = din("consts", [128, 640])
    out_d = nc.dram_tensor("out", [S, D], F32, kind="ExternalOutput").ap()

    X1_d = nc.dram_tensor("X1s", [S, D], F32, kind="ExternalOutput").ap()
    H2_d = nc.dram_tensor("H2s", [S, D], BF16, kind="ExternalOutput").ap()
    Xs_d = nc.dram_tensor("Xs", [256 if os.environ.get("MK_SMALLW") else NBLK * RB, D], BF16, kind="ExternalOutput").ap()
    Ys_d = Xs_d

    with contextlib.ExitStack() as es:
        T = Trk(nc, es)
        _TRK[0] = T

        def dbg(tag, ap, B, ncols, r0=0):
            if tag != dbg_tag:
                return
            T.dma("pool", lambda e: e.dma_start(out=out_d[r0:r0 + ap.shape[0], 0:ncols], in_=ap), reads=[B], writes=[B_dbg], semb=B_dbg)
            T.finish("pool", [B_dbg])
            for en in ("pe", "act", "dve", "sp"):
                T.finish(en, [B_dbg])
            raise _StopBuild()

        B_dbg = T.buf("dbg")

        def sb(stack, name, shape, dt):
            return stack.enter_context(nc.sbuf_tensor("s_" + name, list(shape), dt))

        cst = sb(es, "cst", [128, 640], F32)
        B_cst = T.buf("cst")
        T.dma("sp", lambda e: e.dma_start(out=cst[:], in_=cst_d[:, :]), writes=[B_cst])
        ident_f = cst[:, 0:128]
        iota_p = cst[:, 256:257]
        kbase = cst[:, 257:513]
        ident_b = sb(es, "ident_b", [128, 128], BF16)
        ustrict_b = sb(es, "ustrict_b", [128, 128], BF16)
        ones_b = sb(es, "ones_b", [128, 128], BF16)
        onesA = sb(es, "onesA", [128, 128], BF16)
        onesB = sb(es, "onesB", [128, 128], BF16)
        ones_f = sb(es, "ones_f", [128, 256], F32)
        B_c2 = T.buf("c2")
        fz_t = sb(es, "fz", [128, 4], F32)
        nc.vector.memset(fz_t[:], 0.0)
        T.fence_ap = fz_t
        T.op("dve", lambda e: e.tensor_copy(out=ident_b[:], in_=cst[:, 0:128]), reads=[B_cst], writes=[B_c2])
        T.op("dve", lambda e: e.tensor_copy(out=ustrict_b[:], in_=cst[:, 128:256]), reads=[B_cst], writes=[B_c2])
        T.op("dve", lambda e: e.memset(ones_b[:], 1.0), writes=[B_c2])
        T.op("dve", lambda e: e.memset(ones_f[:], 1.0), writes=[B_c2])
        T.op("dve", lambda e: e.memset(onesA[:], 0.0), writes=[B_c2])
        T.op("dve", lambda e: e.memset(onesB[:], 0.0), writes=[B_c2])
        T.op("dve", lambda e: e.memset(onesA[:, 0:64], 1.0), writes=[B_c2])
        T.op("dve", lambda e: e.memset(onesB[:, 64:128], 1.0), writes=[B_c2])

        rows = sb(es, "rows", [128, 5, D], F32)
        B_rows = T.buf("rows")
        B_fg = T.buf("fgrow")
        wout_b = sb(es, "wout_b", [128, 8, D], BF16)
        B_wout = T.buf("wout")
        key8 = sb(es, "key8", [128, NB, 8], F32)
        w8 = sb(es, "w8", [128, NB, 8], F32)
        c8 = sb(es, "c8", [128, NB, 8], F32)
        B_key8 = T.buf("key8")
        B_w8 = T.buf("w8")
        cntb = sb(es, "cntb", [128, E], F32)
        B_cntb = T.buf("cntb")
        T.op("dve", lambda e: e.memset(cntb[:], 0.0), writes=[B_cntb])

        psf = [es.enter_context(nc.psum_tensor("psf%d" % i, [128, 512], F32)) for i in range(6)]
        psb = [es.enter_context(nc.psum_tensor("psb%d" % i, [128, 1024], BF16)) for i in range(2)]
        B_psf = [T.buf("psf%d" % i) for i in range(6)]
        B_psb = [T.buf("psb%d" % i) for i in range(2)]
        rr = {"f": 0, "b": 0}

        def PF():
            i = rr["f"] % 6
            rr["f"] += 1
            return psf[i], B_psf[i]

        def PB():
            i = rr["b"] % 2
            rr["b"] += 1
            return psb[i], B_psb[i]

        def mmgroup(out_ap, B_out, pairs, reads):
            n = len(pairs)
            for i, (l, r) in enumerate(pairs):
                T.op("pe", lambda e, l=l, r=r, i=i: e.matmul(out_ap, lhsT=l, rhs=r, start=(i == 0), stop=(i == n - 1)),
                     reads=reads, writes=[B_out], sig=(i == n - 1), nowaw=(i > 0))

        cp_rr = [0]

        def evac(out_ap, in_ap, reads, writes, eng=None, nowaw=False):
            if eng is None:
                eng = "act" if cp_rr[0] % 2 == 0 else "dve"
                cp_rr[0] += 1
            if eng == "act":
                T.op("act", lambda e: e.activation(out=out_ap, in_=in_ap, func=AF.Copy), reads=reads, writes=writes, nowaw=nowaw)
            else:
                T.op("dve", lambda e: e.tensor_copy(out=out_ap, in_=in_ap), reads=reads, writes=writes, nowaw=nowaw)

        B_g1 = T.buf("g1row")

        def bcast_row(src_ap, dst_ap, B_src, B_dst, add1):
            for hf in range(2):
                ps, B_ps = PF()
                mmgroup(ps[:, :], B_ps, [(ones_f[0:1, 0:128], src_ap[0:1, hf * 512:(hf + 1) * 512])], [B_src, B_c2])
                if add1:
                    T.op("dve", lambda e, ps=ps, hf=hf: e.tensor_scalar(
                        out=dst_ap[:, hf * 512:(hf + 1) * 512], in0=ps[:, :], scalar1=1.0, scalar2=None, op0=ALU.add),
                        reads=[B_ps], writes=[B_dst], nowaw=True)
                else:
                    T.op("dve", lambda e, ps=ps, hf=hf: e.tensor_copy(out=dst_ap[:, hf * 512:(hf + 1) * 512], in_=ps[:, :]),
                         reads=[B_ps], writes=[B_dst], nowaw=True)

        with contextlib.ExitStack() as e0:
            g1row = sb(e0, "g1row", [128, D], F32)
            stg2 = [sb(e0, "stg2_%d" % i, [128, D], F32) for i in range(2)]
            B_stg2 = [T.buf("stg2_0"), T.buf("stg2_1")]
            cfm = sb(e0, "cfm", [128, 8], F32)
            scf = sb(e0, "scf", [128, 8], F32)
            modrow = sb(e0, "modrow", [1, 6 * D], F32)
            badar = sb(e0, "badar", [1, 6 * D], F32)
            wa = [sb(e0, "wa%d" % i, [128, 8, 512], F32) for i in range(2)]
            B_cfm, B_scf, B_mod, B_bada = [T.buf(n) for n in ("cfm", "scf", "mod", "bada")]
            B_wa = [T.buf("wa0"), T.buf("wa1")]
            T.dma("sp", lambda e: e.dma_start(out=cfm[:], in_=cfm_d[:, :]), writes=[B_cfm])
            T.dma("sp", lambda e: e.dma_start(out=badar[:], in_=bada_d[:, :]), writes=[B_bada])
            T.op("act", lambda e: e.activation(out=scf[:], in_=cfm[:], func=AF.Silu), reads=[B_cfm], writes=[B_scf])
            for cg in range(12):
                w_t, B_w = wa[cg % 2], B_wa[cg % 2]
                T.dma("sp", lambda e, w_t=w_t, cg=cg: e.dma_start(
                    out=w_t[:], in_=wada_d[:, cg * 512:(cg + 1) * 512].rearrange("(kc p) n -> p kc n", p=128)),
                    writes=[B_w])
                ps, B_ps = PF()
                mmgroup(ps[0:1, :], B_ps, [(scf[:, kc:kc + 1], w_t[:, kc, :]) for kc in range(8)], [B_scf, B_w])
                T.op("dve", lambda e, ps=ps, cg=cg: e.tensor_tensor(
                    out=modrow[0:1, cg * 512:(cg + 1) * 512], in0=ps[0:1, :], in1=badar[0:1, cg * 512:(cg + 1) * 512], op=ALU.add),
                    reads=[B_ps, B_bada], writes=[B_mod], nowaw=True)
            bcast_row(modrow[0:1, 1 * D:2 * D], rows[:, 0, :], B_mod, B_rows, True)
            bcast_row(modrow[0:1, 0 * D:1 * D], rows[:, 1, :], B_mod, B_rows, False)
            bcast_row(modrow[0:1, 4 * D:5 * D], rows[:, 2, :], B_mod, B_rows, True)
            bcast_row(modrow[0:1, 3 * D:4 * D], rows[:, 3, :], B_mod, B_rows, False)
            bcast_row(modrow[0:1, 5 * D:6 * D], rows[:, 4, :], B_mod, B_rows, False)
            bcast_row(modrow[0:1, 2 * D:3 * D], g1row[:, :], B_mod, B_g1, False)
            dbg("modrow", modrow[0:1, 0:1024], B_mod, 1024)
            dbg("modrow5", modrow[0:1, 5120:6144], B_mod, 1024)
            dbg("rows0", rows[:, 0, :], B_rows, 1024)
            dbg("rows4", rows[:, 4, :], B_rows, 1024)
            for kc in range(8):
                T.dma("sp", lambda e, kc=kc: e.dma_start(out=stg2[kc % 2][:], in_=wout_d[kc * 128:(kc + 1) * 128, :]),
                      writes=[B_stg2[kc % 2]])
                T.op("dve", lambda e, kc=kc: e.tensor_tensor(out=wout_b[:, kc, :], in0=stg2[kc % 2][:], in1=g1row[:], op=ALU.mult),
                     reads=[B_stg2[kc % 2], B_g1], writes=[B_wout], nowaw=True)
            for en in ("dve", "pe", "act", "sp", "pool"):
                T.finish(en, [B_rows, B_g1, B_wout, B_mod, B_bada, B_cfm, B_scf] + B_stg2 + B_wa)

        with contextlib.ExitStack() as eA:
            win_b = sb(eA, "win_b", [128, 8, 1792], BF16)
            wr_b = sb(eA, "wr_b", [128, 8, E], BF16)
            wsgu_b = sb(eA, "wsgu_b", [128, 8, 512], BF16)
            wsd_b = sb(eA, "wsd_b", [128, 2, D], BF16)
            diag = sb(eA, "diag", [128, 124, 128], BF16)
            biasT = sb(eA, "biasT", [128, 4, 512], BF16)
            smalls = sb(eA, "smalls", [128, 6, 4], F32)
            convw = sb(eA, "convw", [128, 124], F32)
            rbrow = sb(eA, "rbrow", [128, E], F32)
            sinkb = sb(eA, "sinkb", [128, 512], F32)
            B_win, B_wr, B_wsgu, B_wsd, B_diag, B_biasT, B_sm, B_cw, B_rb, B_sinkb = [
                T.buf(n) for n in ("win", "wr", "wsgu", "wsd", "diag", "biasT", "sm", "cw", "rb", "sinkb")]

            for kc in range(8):
                T.dma("pool", lambda e, kc=kc: e.dma_start(out=win_b[:, kc, :], in_=win_d[kc * 128:(kc + 1) * 128, :]),
                      writes=[B_win], nowaw=True)
                T.dma("pool", lambda e, kc=kc: e.dma_start(out=wr_b[:, kc, :], in_=wr_d[kc * 128:(kc + 1) * 128, :]),
                      writes=[B_wr], nowaw=True)
                T.dma("pool", lambda e, kc=kc: e.dma_start(out=wsgu_b[:, kc, 0:256], in_=wsg_d[kc * 128:(kc + 1) * 128, :]),
                      writes=[B_wsgu], nowaw=True)
                T.dma("pool", lambda e, kc=kc: e.dma_start(out=wsgu_b[:, kc, 256:512], in_=wsu_d[kc * 128:(kc + 1) * 128, :]),
                      writes=[B_wsgu], nowaw=True)
            for hc in range(2):
                T.dma("pool", lambda e, hc=hc: e.dma_start(out=wsd_b[:, hc, :], in_=wsd_d[hc * 128:(hc + 1) * 128, :]),
                      writes=[B_wsd], nowaw=True)
            for i, src in enumerate((sinkfm_d, angfm_d, convb_d, lng_d, lnb_d)):
                T.dma("sp", lambda e, i=i, src=src: e.dma_start(out=smalls[:, i, :], in_=src[:, :]), writes=[B_sm], nowaw=True)
            T.dma("sp", lambda e: e.dma_start(out=convw[:], in_=convw_d[:, :]), writes=[B_cw])
            T.op("act", lambda e: e.activation(out=smalls[:, 0, :], in_=smalls[:, 0, :], func=AF.Exp), reads=[B_sm], writes=[B_sm])
            for c in range(4):
                T.op("dve", lambda e, c=c: e.tensor_scalar(out=sinkb[:, c * 128:(c + 1) * 128], in0=ones_f[:, 0:128],
                                                          scalar1=smalls[:, 0, c:c + 1], scalar2=None, op0=ALU.mult),
                     reads=[B_sm, B_c2], writes=[B_sinkb], nowaw=True)
            with contextlib.ExitStack() as e1:
                rbr = sb(e1, "rbr", [1, E], F32)
                B_rbr = T.buf("rbr")
                T.dma("sp", lambda e: e.dma_start(out=rbr[:], in_=rb_d[:, :]), writes=[B_rbr])
                ps, B_ps = PF()
                mmgroup(ps[:, 0:E], B_ps, [(ones_f[0:1, 0:128], rbr[0:1, :])], [B_rbr, B_c2])
                T.op("dve", lambda e: e.tensor_copy(out=rbrow[:], in_=ps[:, 0:E]), reads=[B_ps], writes=[B_rb])
                stg = [sb(e1, "stg%d" % i, [128, D], F32) for i in range(2)]
                B_stg = [T.buf("stg0"), T.buf("stg1")]
                mk = sb(e1, "mk", [128, 512], F32)
                B_mk = T.buf("mk")
                for j in range(2):
                    T.dma("sp", lambda e, j=j: e.dma_start(out=mk[:], in_=maskc_d[j]), writes=[B_mk])
                    for g in range(2):
                        n = j * 2 + g
                        T.dma("sp", lambda e, j=j, g=g, n=n: e.dma_start(out=stg[n % 2][:, 0:512], in_=biasg_d[j, g]),
                              writes=[B_stg[n % 2]])
                        T.op("dve", lambda e, n=n: e.scalar_tensor_tensor(
                            out=biasT[:, n, :], in0=stg[n % 2][:, 0:512], scalar=8.0, in1=mk[:],
                            op0=ALU.mult, op1=ALU.add),
                            reads=[B_stg[n % 2], B_mk], writes=[B_biasT], nowaw=True)
                for en in ("dve", "sp", "pe", "act", "pool"):
                    T.finish(en, [B_biasT, B_mk, B_rbr, B_rb] + B_stg)
            for c in range(4):
                for j in range(31):
                    T.op("dve", lambda e, c=c, j=j: e.tensor_scalar(
                        out=diag[:, c * 31 + j, :], in0=cst[:, 0:128], scalar1=convw[:, c * 31 + j:c * 31 + j + 1],
                        scalar2=None, op0=ALU.mult), reads=[B_cw, B_cst], writes=[B_diag], nowaw=True)

            xt = sb(eA, "xt", [128, 4, D], F32)
            htok = sb(eA, "htok", [128, D], BF16)
            hT = sb(eA, "hT", [128, 8, 512], BF16)
            tmpf = sb(eA, "tmpf", [128, D], F32)
            den = tmpf[:, 0:512]
            on = tmpf[:, 512:1024]
            junk = sb(eA, "junk", [128, D], BF16)
            st = sb(eA, "st", [128, 16], F32)
            qT = sb(eA, "qT", [128, 4, 512], BF16)
            kT = sb(eA, "kT", [128, 640], BF16)
            vA = sb(eA, "vA", [128, 5, 128], BF16)
            vB = sb(eA, "vB", [128, 5, 128], BF16)
            uT = sb(eA, "uT", [128, 4, 544], BF16)
            sg = sb(eA, "sg", [128, 512], F32)
            cvf = sb(eA, "cvf", [128, 4, 512], F32)
            mstat = sb(eA, "mstat", [128, 2, 512], F32)
            catT = sb(eA, "catT", [128, 8, 512], BF16)
            pT = sb(eA, "pT", [128, 4, 512], BF16)
            sqb = pT
            cvb = catT
            sqa = sb(eA, "sqa", [128, 512], BF16)
            rsa = sb(eA, "rsa", [128, 128], F32)
            shT = sb(eA, "shT", [128, 2, 512], BF16)
            rt = sb(eA, "rt", [128, 6, E], F32)
            emb = sb(eA, "emb", [128, E], BF16)
            m8 = sb(eA, "m8", [128, 12, 8], F32)
            (B_xt, B_htok, B_hT, B_tmpf, B_junk, B_st, B_qT, B_kT, B_vA, B_vB, B_uT, B_sg, B_cvf, B_cvb, B_sqb, B_mstat,
             B_catT, B_pT, B_den, B_on, B_sqa, B_rsa, B_shT, B_rt, B_emb, B_m8, B_X1, B_H2) = [
                T.buf(n) for n in ("xt htok hT tmpf junk st qT kT vA vB uT sg cvf cvb sqb mstat catT pT den on sqa rsa "
                                   "shT rt emb m8 X1 H2").split()]
            B_den = B_on = B_tmpf
            B_cvb = B_catT
            B_sqb = B_pT
            T.op("dve", lambda e: e.memset(vA[:], 0.0), writes=[B_vA])
            T.op("dve", lambda e: e.memset(vB[:], 0.0), writes=[B_vB])
            T.op("dve", lambda e: e.memset(uT[:], 0.0), writes=[B_uT])
            T.op("dve", lambda e: e.memset(kT[:], 0.0), writes=[B_kT])

            def norm_mod(src, B_src, ri, h2_t=None):
                for blk in range(4):
                    T.op("act", lambda e, blk=blk: e.activation(out=junk[:], in_=src[:, blk, :], func=AF.Square,
                                                              accum_out=st[:, blk:blk + 1]),
                         reads=[B_src], writes=[B_junk, B_st])
                T.op("dve", lambda e: e.tensor_scalar(out=st[:, 4:8], in0=st[:, 0:4], scalar1=1.0 / D, scalar2=EPS,
                                                      op0=ALU.mult, op1=ALU.add), reads=[B_st], writes=[B_st])
                T.op("act", lambda e: e.activation(out=st[:, 12:16], in_=st[:, 4:8], func=AF.Sqrt), reads=[B_st], writes=[B_st])
                T.op("dve", lambda e: e.reciprocal(out=st[:, 8:12], in_=st[:, 12:16]), reads=[B_st], writes=[B_st], fence=True)
                for blk in range(4):
                    T.op("dve", lambda e, blk=blk: e.scalar_tensor_tensor(
                        out=tmpf[:], in0=src[:, blk, :], scalar=st[:, 8 + blk:9 + blk], in1=rows[:, ri, :],
                        op0=ALU.mult, op1=ALU.mult), reads=[B_src, B_st, B_rows], writes=[B_tmpf])
                    T.op("dve", lambda e: e.tensor_tensor(out=htok[:], in0=tmpf[:], in1=rows[:, ri + 1, :], op=ALU.add),
                         reads=[B_tmpf, B_rows], writes=[B_htok])
                    if h2_t is not None:
                        gb = h2_t * 4 + blk
                        T.dma("sp", lambda e, gb=gb: e.dma_start(out=H2_d[gb * 128:(gb + 1) * 128, :], in_=htok[:]),
                              reads=[B_htok], writes=[B_H2], semb=B_htok, nowaw=True)
                    pb, B_pb = PB()
                    for kc in range(8):
                        T.op("pe", lambda e, kc=kc, pb=pb: e.transpose(
                            out=pb[:, kc * 128:(kc + 1) * 128], in_=htok[:, kc * 128:(kc + 1) * 128], identity=ident_b[:]),
                            reads=[B_htok, B_c2], writes=[B_pb], sig=(kc == 7), nowaw=(kc > 0))
                    evac(hT[:, :, blk * 128:(blk + 1) * 128], pb[:, :].rearrange("p (a b) -> p a b", a=8), [B_pb], [B_hT],
                         nowaw=(blk > 0))

            for t in range(int(os.environ.get("MK_NT", NT)) if stage >= 1 else 0):
                T.dma("sp", lambda e, t=t: e.dma_start(
                    out=xt[:], in_=x_d[t * 512:(t + 1) * 512, :].rearrange("(b p) n -> p b n", p=128)), writes=[B_xt])
                dbg("rows0b", rows[:, 0, :], B_rows, 1024)
                dbg("rows1b", rows[:, 1, :], B_rows, 1024)
                norm_mod(xt, B_xt, 0)
                dbg("st", st[:, :], B_st, 16)
                dbg("tmpf", tmpf[:], B_tmpf, 1024)
                dbg("htok", htok[:], B_htok, 1024)
                dbg("hT0", hT[:, 0, :], B_hT, 512)
                for c in range(4):
                    ps, B_ps = PF()
                    mmgroup(ps[:, :], B_ps, [(win_b[:, kc, c * 128:(c + 1) * 128], hT[:, kc, :]) for kc in range(8)], [B_win, B_hT])
                    evac(qT[:, c, :], ps[:, :], [B_ps], [B_qT])
                ps, B_ps = PF()
                mmgroup(ps[:, :], B_ps, [(win_b[:, kc, 512:640], hT[:, kc, :]) for kc in range(8)], [B_win, B_hT])
                evac(kT[:, 128:640], ps[:, :], [B_ps], [B_kT])
                for blk in range(4):
                    ps, B_ps = PF()
                    mmgroup(ps[:, 0:128], B_ps, [(hT[:, kc, blk * 128:(blk + 1) * 128], win_b[:, kc, 640:768]) for kc in range(8)],
                            [B_win, B_hT])
                    T.op("act", lambda e, ps=ps, blk=blk: e.activation(out=vA[:, blk + 1, 0:64], in_=ps[:, 0:64], func=AF.Copy),
                         reads=[B_ps], writes=[B_vA])
                    T.op("dve", lambda e, ps=ps, blk=blk: e.tensor_copy(out=vB[:, blk + 1, 64:128], in_=ps[:, 64:128]),
                         reads=[B_ps], writes=[B_vB])
                for c in range(4):
                    psa, B_psa = PF()
                    mmgroup(psa[:, :], B_psa, [(win_b[:, kc, 768 + c * 128:768 + (c + 1) * 128], hT[:, kc, :]) for kc in range(8)],
                            [B_win, B_hT])
                    psg, B_psg = PF()
                    mmgroup(psg[:, :], B_psg, [(win_b[:, kc, 1280 + c * 128:1280 + (c + 1) * 128], hT[:, kc, :]) for kc in range(8)],
                            [B_win, B_hT])
                    T.op("act", lambda e, psg=psg: e.activation(out=sg[:], in_=psg[:, :], func=AF.Sigmoid), reads=[B_psg], writes=[B_sg])
                    T.op("dve", lambda e, psa=psa, c=c: e.tensor_tensor(out=uT[:, c, 30:542], in0=psa[:, :], in1=sg[:], op=ALU.mult),
                         reads=[B_psa, B_sg], writes=[B_uT])
                dbg("qT0", qT[:, 0, :], B_qT, 512)
                dbg("kT", kT[:, 128:640], B_kT, 512)
                dbg("vA", vA[:, 1, :], B_vA, 128)
                dbg("uT0", uT[:, 0, 30:542], B_uT, 512)
                for c in range(4):
                    ps, B_ps = PF()
                    mmgroup(ps[:, :], B_ps, [(diag[:, c * 31 + j, :], uT[:, c, j:j + 512]) for j in range(31)], [B_diag, B_uT])
                    T.op("act", lambda e, ps=ps, c=c: e.activation(out=cvf[:, c, :], in_=ps[:, :], func=AF.Identity,
                                                                 bias=smalls[:, 2, c:c + 1]), reads=[B_ps, B_sm], writes=[B_cvf])
                    T.op("act", lambda e, ps=ps, c=c: e.activation(out=sqb[:, c, :], in_=ps[:, :], func=AF.Square,
                                                                 bias=smalls[:, 2, c:c + 1]), reads=[B_ps, B_sm], writes=[B_sqb])
                    T.op("dve", lambda e, c=c: e.tensor_copy(out=cvb[:, c, :], in_=cvf[:, c, :]), reads=[B_cvf], writes=[B_cvb])
                    T.op("dve", lambda e, c=c: e.tensor_copy(out=uT[:, c, 0:30], in_=uT[:, c, 512:542]), reads=[B_uT], writes=[B_uT])
                ps1, B_ps1 = PF()
                mmgroup(ps1[:, :], B_ps1, [(ones_b[:], cvb[:, c, :]) for c in range(4)], [B_cvb, B_c2])
                ps2, B_ps2 = PF()
                mmgroup(ps2[:, :], B_ps2, [(ones_b[:], sqb[:, c, :]) for c in range(4)], [B_sqb, B_c2])
                T.op("dve", lambda e: e.tensor_scalar(out=mstat[:, 0, :], in0=ps1[:, :], scalar1=1.0 / 512, scalar2=None, op0=ALU.mult),
                     reads=[B_ps1], writes=[B_mstat])
                T.op("dve", lambda e: e.tensor_tensor(out=sg[:], in0=mstat[:, 0, :], in1=mstat[:, 0, :], op=ALU.mult),
                     reads=[B_mstat], writes=[B_sg])
                T.op("dve", lambda e: e.scalar_tensor_tensor(out=mstat[:, 1, :], in0=ps2[:, :], scalar=1.0 / 512, in1=sg[:],
                                                             op0=ALU.mult, op1=ALU.subtract), reads=[B_ps2, B_sg], writes=[B_mstat])
                T.op("dve", lambda e: e.tensor_scalar(out=mstat[:, 1, :], in0=mstat[:, 1, :], scalar1=EPS, scalar2=None,
                                                      op0=ALU.add), reads=[B_mstat], writes=[B_mstat])
                T.op("act", lambda e: e.activation(out=mstat[:, 1, :], in_=mstat[:, 1, :], func=AF.Sqrt), reads=[B_mstat], writes=[B_mstat])
                T.op("dve", lambda e: e.reciprocal(out=mstat[:, 1, :], in_=mstat[:, 1, :]), reads=[B_mstat], writes=[B_mstat])
                for c in range(4):
                    T.op("dve", lambda e, c=c: e.tensor_tensor(out=cvf[:, c, :], in0=cvf[:, c, :], in1=mstat[:, 0, :], op=ALU.subtract),
                         reads=[B_cvf, B_mstat], writes=[B_cvf])
                    T.op("dve", lambda e, c=c: e.tensor_tensor(out=cvf[:, c, :], in0=cvf[:, c, :], in1=mstat[:, 1, :], op=ALU.mult),
                         reads=[B_cvf, B_mstat], writes=[B_cvf])
                    T.op("act", lambda e, c=c: e.activation(out=catT[:, 4 + c, :], in_=cvf[:, c, :], func=AF.Silu,
                                                          scale=smalls[:, 3, c:c + 1], bias=smalls[:, 4, c:c + 1]),
                         reads=[B_cvf, B_sm], writes=[B_catT], nowaw=True)
                dbg("cvf0", cvf[:, 0, :], B_cvf, 512)
                dbg("conv0", catT[:, 4, :], B_catT, 512)
                for blk in range(4):
                    gb = t * 4 + blk
                    js = (1,) if gb == 0 else (0, 1)
                    for g in range(2):
                        for j in js:
                            ps, B_ps = PF()
                            kcol = blk * 128 + j * 128
                            pairs = [(ident_b[:], biasT[:, j * 2 + g, :])]
                            T.op("pe", lambda e, ps=ps, j=j, g=g: e.matmul(ps[:, :], lhsT=ident_b[:], rhs=biasT[:, j * 2 + g, :],
                                                                          start=True, stop=False),
                                 reads=[B_c2, B_biasT], writes=[B_ps], sig=False)
                            for i in range(4):
                                T.op("pe", lambda e, ps=ps, i=i, g=g, kcol=kcol, blk=blk: e.matmul(
                                    ps[:, i * 128:(i + 1) * 128], lhsT=kT[g * 64:(g + 1) * 64, kcol:kcol + 128],
                                    rhs=qT[g * 64:(g + 1) * 64, i, blk * 128:(blk + 1) * 128], start=False, stop=(i == 3)),
                                    reads=[B_kT, B_qT], writes=[B_ps], sig=(i == 3), nowaw=True)
                            T.op("act", lambda e, ps=ps, g=g, j=j: e.activation(out=pT[:, g * 2 + j, :], in_=ps[:, :], func=AF.Exp, scale=0.125),
                                 reads=[B_ps], writes=[B_pT], nowaw=True)
                    pso, B_pso = PF()
                    psd, B_psd = PF()
                    for c in range(4):
                        prs_o = []
                        prs_d = []
                        for j in js:
                            vs = blk + j
                            prs_o.append((vA[:, vs, :], pT[:, 0 * 2 + j, c * 128:(c + 1) * 128]))
                            prs_o.append((vB[:, vs, :], pT[:, 1 * 2 + j, c * 128:(c + 1) * 128]))
                            prs_d.append((onesA[:], pT[:, 0 * 2 + j, c * 128:(c + 1) * 128]))
                            prs_d.append((onesB[:], pT[:, 1 * 2 + j, c * 128:(c + 1) * 128]))
                        n = len(prs_o)
                        for i2, (l, r) in enumerate(prs_o):
                            T.op("pe", lambda e, l=l, r=r, i2=i2, c=c: e.matmul(pso[:, c * 128:(c + 1) * 128], lhsT=l, rhs=r,
                                                                                start=(i2 == 0), stop=(i2 == n - 1)),
                                 reads=[B_vA, B_vB, B_pT], writes=[B_pso], sig=(i2 == n - 1 and c == 3), nowaw=not (c == 0 and i2 == 0))
                        for i2, (l, r) in enumerate(prs_d):
                            T.op("pe", lambda e, l=l, r=r, i2=i2, c=c: e.matmul(psd[:, c * 128:(c + 1) * 128], lhsT=l, rhs=r,
                                                                                start=(i2 == 0), stop=(i2 == n - 1)),
                                 reads=[B_c2, B_pT], writes=[B_psd], sig=(i2 == n - 1 and c == 3), nowaw=not (c == 0 and i2 == 0))
                    T.op("dve", lambda e: e.tensor_tensor(out=den[:], in0=psd[:, :], in1=sinkb[:], op=ALU.add),
                         reads=[B_psd, B_sinkb], writes=[B_den])
                    T.op("dve", lambda e: e.reciprocal(out=den[:], in_=den[:]), reads=[B_den], writes=[B_den])
                    T.op("dve", lambda e: e.tensor_tensor(out=on[:], in0=pso[:, :], in1=den[:], op=ALU.mult),
                         reads=[B_pso, B_den], writes=[B_on])
                    T.op("act", lambda e: e.activation(out=sqa[:], in_=on[:], func=AF.Square), reads=[B_on], writes=[B_sqa])
                    pss, B_pss = PF()
                    mmgroup(pss[:, 0:128], B_pss, [(ones_b[:], sqa[:, c * 128:(c + 1) * 128]) for c in range(4)], [B_sqa, B_c2])
                    T.op("dve", lambda e: e.tensor_scalar(out=rsa[:], in0=pss[:, 0:128], scalar1=1.0 / 512, scalar2=EPS,
                                                          op0=ALU.mult, op1=ALU.add), reads=[B_pss], writes=[B_rsa])
                    T.op("act", lambda e: e.activation(out=rsa[:], in_=rsa[:], func=AF.Sqrt), reads=[B_rsa], writes=[B_rsa])
                    T.op("dve", lambda e: e.reciprocal(out=rsa[:], in_=rsa[:]), reads=[B_rsa], writes=[B_rsa])
                    for c in range(4):
                        T.op("dve", lambda e, c=c, blk=blk: e.scalar_tensor_tensor(
                            out=catT[:, c, blk * 128:(blk + 1) * 128], in0=on[:, c * 128:(c + 1) * 128], scalar=smalls[:, 1, c:c + 1],
                            in1=rsa[:], op0=ALU.mult, op1=ALU.mult), reads=[B_on, B_sm, B_rsa], writes=[B_catT], nowaw=True)
                dbg("attn0", catT[:, 0, :], B_catT, 512)
                T.op("act", lambda e: e.activation(out=kT[:, 0:128], in_=kT[:, 512:640], func=AF.Copy), reads=[B_kT], writes=[B_kT])
                T.op("act", lambda e: e.activation(out=vA[:, 0, :], in_=vA[:, 4, :], func=AF.Copy), reads=[B_vA], writes=[B_vA])
                T.op("act", lambda e: e.activation(out=vB[:, 0, :], in_=vB[:, 4, :], func=AF.Copy), reads=[B_vB], writes=[B_vB])
                for blk in range(4):
                    for hf in range(2):
                        ps, B_ps = PF()
                        mmgroup(ps[:, :], B_ps, [(catT[:, kc, blk * 128:(blk + 1) * 128], wout_b[:, kc, hf * 512:(hf + 1) * 512])
                                                 for kc in range(8)], [B_catT, B_wout])
                        T.op("dve", lambda e, ps=ps, blk=blk, hf=hf: e.tensor_tensor(
                            out=xt[:, blk, hf * 512:(hf + 1) * 512], in0=ps[:, :], in1=xt[:, blk, hf * 512:(hf + 1) * 512], op=ALU.add),
                            reads=[B_ps, B_xt], writes=[B_xt], nowaw=True)
                if stage == 1:
                    T.dma("sp", lambda e, t=t: e.dma_start(
                        out=out_d[t * 512:(t + 1) * 512, :].rearrange("(b p) n -> p b n", p=128), in_=xt[:]),
                        reads=[B_xt], writes=[B_X1], semb=B_xt, nowaw=True)
                    continue
                norm_mod(xt, B_xt, 2, h2_t=t)
                for hc in range(4):
                    ps, B_ps = PF()
                    mmgroup(ps[:, :], B_ps, [(wsgu_b[:, kc, hc * 128:(hc + 1) * 128], hT[:, kc, :]) for kc in range(8)], [B_wsgu, B_hT])
                    if hc < 2:
                        T.op("act", lambda e, ps=ps, hc=hc: e.activation(out=cvf[:, hc, :], in_=ps[:, :], func=AF.Silu),
                             reads=[B_ps], writes=[B_cvf])
                    else:
                        T.op("dve", lambda e, ps=ps, hc=hc: e.tensor_tensor(out=shT[:, hc - 2, :], in0=ps[:, :], in1=cvf[:, hc - 2, :],
                                                                            op=ALU.mult), reads=[B_ps, B_cvf], writes=[B_shT], nowaw=True)
                for blk in range(4):
                    gb = t * 4 + blk
                    for hf in range(2):
                        ps, B_ps = PF()
                        mmgroup(ps[:, :], B_ps, [(shT[:, hc, blk * 128:(blk + 1) * 128], wsd_b[:, hc, hf * 512:(hf + 1) * 512])
                                                 for hc in range(2)], [B_shT, B_wsd])
                        T.op("dve", lambda e, ps=ps, hf=hf: e.tensor_tensor(out=tmpf[:, 0:512], in0=ps[:, :],
                                                                            in1=rows[:, 4, hf * 512:(hf + 1) * 512], op=ALU.mult),
                             reads=[B_ps, B_rows], writes=[B_tmpf])
                        T.op("dve", lambda e, blk=blk, hf=hf: e.tensor_tensor(
                            out=xt[:, blk, hf * 512:(hf + 1) * 512], in0=tmpf[:, 0:512], in1=xt[:, blk, hf * 512:(hf + 1) * 512], op=ALU.add),
                            reads=[B_tmpf, B_xt], writes=[B_xt], nowaw=True)
                    ps, B_ps = PF()
                    mmgroup(ps[:, 0:E], B_ps, [(hT[:, kc, blk * 128:(blk + 1) * 128], wr_b[:, kc, :]) for kc in range(8)], [B_hT, B_wr])
                    sc_, sel, selm, gw, keyt, jk = [rt[:, i, :] for i in range(6)]
                    T.op("act", lambda e, ps=ps: e.activation(out=rt[:, 0, :], in_=ps[:, 0:E], func=AF.Sigmoid), reads=[B_ps], writes=[B_rt])
                    R = dict(reads=[B_rt, B_m8, B_rb, B_emb, B_cst, B_cntb], writes=[B_rt, B_m8])
                    T.op("dve", lambda e: e.tensor_tensor(out=sel, in0=sc_, in1=rbrow[:], op=ALU.add), **R)
                    for g in range(8):
                        T.op("dve", lambda e, g=g: e.max(out=m8[:, g, :], in_=rt[:, 1, g * 32:(g + 1) * 32]), fence=(g == 7), **R)
                    T.op("dve", lambda e: e.tensor_tensor(out=m8[:, 8, :], in0=m8[:, 0:8, 0], in1=m8[:, 0:8, 1], op=ALU.add), fence=True, **R)
                    T.op("dve", lambda e: e.max(out=m8[:, 9, :], in_=m8[:, 8, :]), **R, fence=True)
                    T.op("dve", lambda e: e.tensor_scalar(out=m8[:, 10, :], in0=m8[:, 8, :], scalar1=m8[:, 9, 3:4], scalar2=-1e9,
                                                          op0=ALU.is_lt, op1=ALU.mult), fence=True, **R)
                    for g in range(8):
                        T.op("dve", lambda e, g=g: e.tensor_scalar(out=rt[:, 2, g * 32:(g + 1) * 32], in0=rt[:, 1, g * 32:(g + 1) * 32],
                                                                  scalar1=m8[:, 10, g:g + 1], scalar2=None, op0=ALU.add), fence=(g == 7), **R)
                    T.op("dve", lambda e: e.max(out=m8[:, 11, :], in_=selm), **R, fence=True)
                    T.op("dve", lambda e: e.tensor_scalar(out=emb[:], in0=selm, scalar1=m8[:, 11, 7:8], scalar2=None, op0=ALU.is_ge),
                         reads=[B_rt, B_m8], writes=[B_emb], fence=True)
                    T.op("dve", lambda e: e.scalar_tensor_tensor(out=gw, in0=sc_, scalar=1.0, in1=emb[:], op0=ALU.mult, op1=ALU.mult,
                                                                 accum_out=m8[:, 9, 0:1]), **R, fence=True)
                    T.op("dve", lambda e: e.reciprocal(out=m8[:, 9, 1:2], in_=m8[:, 9, 0:1]), fence=True, **R)
                    psr, B_psr = PF()
                    mmgroup(psr[:, 0:E], B_psr, [(ustrict_b[:], emb[:])], [B_emb, B_c2])
                    psc, B_psc = PF()
                    mmgroup(psc[:, 0:E], B_psc, [(ones_b[:], emb[:])], [B_emb, B_c2])
                    T.op("dve", lambda e, psr=psr: e.tensor_tensor(out=keyt, in0=psr[:, 0:E], in1=cntb[:], op=ALU.add),
                         reads=[B_psr, B_cntb, B_rt], writes=[B_rt])
                    T.op("dve", lambda e, psc=psc: e.tensor_tensor(out=cntb[:], in0=psc[:, 0:E], in1=cntb[:], op=ALU.add),
                         reads=[B_psc, B_cntb, B_rt], writes=[B_cntb])
                    T.op("dve", lambda e: e.tensor_tensor(out=sel, in0=kbase, in1=emb[:], op=ALU.mult), fence=True, **R)
                    T.op("dve", lambda e, gb=gb: e.max(out=key8[:, gb, :], in_=sel), reads=[B_rt], writes=[B_key8], nowaw=True, fence=True)
                    for k in range(8):
                        T.op("dve", lambda e, k=k, gb=gb: e.scalar_tensor_tensor(
                            out=jk, in0=sel, scalar=key8[:, gb, k:k + 1], in1=gw, op0=ALU.is_equal, op1=ALU.mult,
                            accum_out=w8[:, gb, k:k + 1]), reads=[B_rt, B_key8], writes=[B_rt, B_w8], nowaw=True)
                        T.op("dve", lambda e, k=k, gb=gb: e.scalar_tensor_tensor(
                            out=jk, in0=sel, scalar=key8[:, gb, k:k + 1], in1=keyt, op0=ALU.is_equal, op1=ALU.mult,
                            accum_out=c8[:, gb, k:k + 1]), reads=[B_rt, B_key8], writes=[B_rt, B_w8], nowaw=True, fence=(k == 7))
                    T.op("dve", lambda e, gb=gb: e.tensor_scalar(out=w8[:, gb, :], in0=w8[:, gb, :], scalar1=m8[:, 9, 1:2], scalar2=2.5,
                                                                op0=ALU.mult, op1=ALU.mult), reads=[B_m8, B_w8], writes=[B_w8], nowaw=True)
                T.dma("sp", lambda e, t=t: e.dma_start(
                    out=X1_d[t * 512:(t + 1) * 512, :].rearrange("(b p) n -> p b n", p=128), in_=xt[:]),
                    reads=[B_xt], writes=[B_X1], semb=B_xt, nowaw=True)
            for en in ("pe", "act", "dve", "pool", "sp"):
                T.finish(en, [B_xt, B_htok, B_hT, B_catT, B_rt, B_key8, B_w8, B_cntb, B_X1, B_H2, B_win, B_wout, B_wr, B_wsgu,
                              B_wsd, B_diag, B_biasT, B_kT, B_vA, B_vB, B_uT, B_pT, B_cvf, B_tmpf, B_emb, B_m8, B_junk, B_st,
                              B_sm, B_cw, B_rb, B_sinkb, B_qT, B_sg, B_mstat, B_sqa, B_rsa, B_shT, B_rows] + B_psf + B_psb)

        if stage == 1:
            T.finish("sp", [B_X1])
            return nc

        dest = sb(es, "dest", [128, NB, 8], I32)
        widx = sb(es, "widx", [128, NBLK], I32)
        B_dest, B_widx, B_Xs = T.buf("dest"), T.buf("widx"), T.buf("Xs")
        B_Ys = B_Xs
        with contextlib.ExitStack() as eD:
            tb = sb(eD, "tb", [128, 6, E], F32)
            ebf = sb(eD, "ebf", [128, NBLK], F32)
            d8 = sb(eD, "d8", [128, 4, 8], F32)
            ti = sb(eD, "ti", [128, E], I32)
            hrow = [sb(eD, "hrow%d" % i, [128, D], BF16) for i in range(2)]
            B_tb, B_ebf, B_d8 = T.buf("tb"), T.buf("ebf"), T.buf("d8")
            B_hrow = [T.buf("hrow0"), T.buf("hrow1")]
            RT = dict(reads=[B_tb, B_cntb, B_c2, B_cst], writes=[B_tb])
            T.op("dve", lambda e: e.tensor_scalar(out=tb[:, 0, :], in0=cntb[:], scalar1=float(RB - 1), scalar2=None, op0=ALU.add), **RT)
            T.op("dve", lambda e: e.tensor_copy(out=ti[:], in_=tb[:, 0, :]), **RT)
            T.op("dve", lambda e: e.tensor_scalar(out=ti[:], in0=ti[:], scalar1=8, scalar2=None, op0=ALU.arith_shift_right), **RT)
            T.op("dve", lambda e: e.tensor_copy(out=tb[:, 0, :], in_=ti[:]), **RT)
            T.op("dve", lambda e: e.tensor_tensor_scan(out=tb[:, 2, :], data0=ones_f[:, 0:E], data1=tb[:, 0, :], initial=0.0,
                                                       op0=ALU.mult, op1=ALU.add), **RT)
            T.op("dve", lambda e: e.tensor_tensor(out=tb[:, 3, :], in0=tb[:, 2, :], in1=tb[:, 0, :], op=ALU.subtract), **RT)
            T.op("dve", lambda e: e.tensor_scalar(out=tb[:, 3, :], in0=tb[:, 3, :], scalar1=float(RB), scalar2=None, op0=ALU.mult), **RT)
            dbg("cntb", cntb[:], B_cntb, 256)
            dbg("pend", tb[:, 2, :], B_tb, 256)
            dbg("key8", key8[:, 0, :], B_key8, 8)
            dbg("w8", w8[:, 0, :], B_w8, 8)
            for b in range(NBLK):
                T.op("dve", lambda e, b=b: e.tensor_scalar(out=tb[:, 4, :], in0=tb[:, 2, :], scalar1=float(b), scalar2=0.0,
                                                          op0=ALU.is_le, op1=ALU.add, accum_out=ebf[:, b:b + 1]),
                     reads=[B_tb], writes=[B_tb, B_ebf], nowaw=True, fence=(b == NBLK - 1))
            T.op("dve", lambda e: e.tensor_scalar(out=ebf[:], in0=ebf[:], scalar1=128.0, scalar2=iota_p, op0=ALU.mult, op1=ALU.add),
                 reads=[B_ebf, B_cst], writes=[B_ebf], fence=True)
            T.op("dve", lambda e: e.tensor_copy(out=widx[:], in_=ebf[:]), reads=[B_ebf], writes=[B_widx])
            dbg("ebf", ebf[:], B_ebf, NBLK)
            for gb in range(NB):
                RD = dict(reads=[B_d8, B_key8, B_tb, B_cst], writes=[B_d8, B_tb])
                for k in range(8):
                    T.op("dve", lambda e, k=k, gb=gb: e.scalar_tensor_tensor(
                        out=tb[:, 5, :], in0=kbase, scalar=key8[:, gb, k:k + 1], in1=tb[:, 3, :], op0=ALU.is_equal, op1=ALU.mult,
                        accum_out=d8[:, 2, k:k + 1]), fence=(k == 7), **RD)
                T.op("dve", lambda e, gb=gb: e.tensor_tensor(out=d8[:, 3, :], in0=d8[:, 2, :], in1=c8[:, gb, :], op=ALU.add), fence=True, **RD)
                T.op("dve", lambda e, gb=gb: e.tensor_copy(out=dest[:, gb, :], in_=d8[:, 3, :]), reads=[B_d8], writes=[B_dest], nowaw=True, fence=True)
                if gb == 0:
                    dbg("d8", d8[:, 3, :], B_d8, 8)
                hr, B_hr = hrow[gb % 2], B_hrow[gb % 2]
                T.dma("sp", lambda e, gb=gb, hr=hr: e.dma_start(out=hr[:], in_=H2_d[gb * 128:(gb + 1) * 128, :]),
                      reads=[B_H2], writes=[B_hr])
                for k in range(8):
                    T.dma("pool", lambda e, gb=gb, k=k, hr=hr: e.indirect_dma_start(
                        out=Xs_d[:, :], out_offset=bass.IndirectOffsetOnAxis(ap=dest[:, gb, k:k + 1], axis=0),
                        in_=hr[:], in_offset=None, oob_is_err=False), reads=[B_hr, B_dest], writes=[B_Xs], semb=B_hr, nowaw=True)
            for en in ("pe", "act", "dve", "pool", "sp"):
                T.finish(en, [B_Xs, B_dest, B_widx, B_tb, B_d8, B_ebf] + B_hrow)

        with contextlib.ExitStack() as eB:
            NW = 2
            wg = [sb(eB, "wg%d" % i, [128, 2048], BF16) for i in range(NW)]
            wu = [sb(eB, "wu%d" % i, [128, 2048], BF16) for i in range(NW)]
            wdn = [sb(eB, "wdn%d" % i, [128, 2048], BF16) for i in range(NW)]
            xb = [sb(eB, "xb%d" % i, [128, 2, D], BF16) for i in range(2)]
            yb = [sb(eB, "yb%d" % i, [128, 2, D], BF16) for i in range(2)]
            xT = sb(eB, "xT", [128, 8, 128], BF16)
            sgl = sb(eB, "sgl", [128, 256], F32)
            hid = sb(eB, "hid", [128, 256], BF16)
            hTt = sb(eB, "hTt", [128, 2, 128], BF16)
            B_wgu = [T.buf("wgu%d" % i) for i in range(NW)]
            B_wu = [T.buf("wu%d" % i) for i in range(NW)]
            B_wdn = [T.buf("wdn%d" % i) for i in range(NW)]
            B_xb = [T.buf("xb0"), T.buf("xb1")]
            B_yb = [T.buf("yb0"), T.buf("yb1")]
            B_xT, B_sgl, B_hid, B_hTt = T.buf("xT"), T.buf("sgl"), T.buf("hid"), T.buf("hTt")

            def load_block(b):
                i = b % NW
                io = lambda b=b: bass.IndirectOffsetOnAxis(ap=widx[:, b:b + 1], axis=0)
                T.dma("pool", lambda e: e.indirect_dma_start(
                    out=wg[i][:], out_offset=None, in_=weg_d[:, :], in_offset=io(), oob_is_err=False),
                    reads=[B_widx], writes=[B_wgu[i]])
                T.dma("pool", lambda e: e.indirect_dma_start(
                    out=wu[i][:], out_offset=None, in_=weu_d[:, :], in_offset=io(), oob_is_err=False),
                    reads=[B_widx], writes=[B_wu[i]])
                T.dma("pool", lambda e: e.indirect_dma_start(
                    out=wdn[i][:], out_offset=None, in_=wed_d[:, :], in_offset=io(), oob_is_err=False),
                    reads=[B_widx], writes=[B_wdn[i]])
                T.dma("sp", lambda e: e.dma_start(
                    out=xb[b % 2][:], in_=Xs_d[b * RB:(b + 1) * RB, :].rearrange("(s p) n -> p s n", p=128)),
                    reads=[B_Xs], writes=[B_xb[b % 2]])

            nblk = NBLK if stage >= 3 else 0
            if nblk:
                load_block(0)
            for b in range(nblk):
                if b + 1 < nblk:
                    load_block(b + 1)
                i = b % NW
                for s_ in range(2):
                    pb, B_pb = PB()
                    for j in range(8):
                        T.op("pe", lambda e, j=j, s_=s_: e.transpose(out=pb[:, j * 128:(j + 1) * 128],
                                                                   in_=xb[b % 2][:, s_, j::8], identity=ident_b[:]),
                             reads=[B_xb[b % 2], B_c2], writes=[B_pb], sig=(j == 7), nowaw=(j > 0))
                    evac(xT[:, :, :], pb[:, :].rearrange("p (a b) -> p a b", a=8), [B_pb], [B_xT], eng="dve")
                    ps, B_ps = PF()
                    mmgroup(ps[:, 0:256], B_ps, [(xT[:, j, :], wg[i][:, j * 256:(j + 1) * 256]) for j in range(8)], [B_xT, B_wgu[i]])
                    for j in range(8):
                        T.op("pe", lambda e, j=j, ps=ps: e.matmul(ps[:, 256:512], lhsT=xT[:, j, :], rhs=wu[i][:, j * 256:(j + 1) * 256],
                                                                 start=(j == 0), stop=(j == 7)),
                             reads=[B_xT, B_wu[i]], writes=[B_ps], sig=(j == 7), nowaw=True)
                    T.op("act", lambda e, ps=ps: e.activation(out=sgl[:], in_=ps[:, 0:256], func=AF.Silu), reads=[B_ps], writes=[B_sgl])
                    T.op("dve", lambda e, ps=ps: e.tensor_tensor(out=hid[:], in0=ps[:, 256:512], in1=sgl[:], op=ALU.mult),
                         reads=[B_ps, B_sgl], writes=[B_hid])
                    pb2, B_pb2 = PB()
                    for j in range(2):
                        T.op("pe", lambda e, j=j: e.transpose(out=pb2[:, j * 128:(j + 1) * 128], in_=hid[:, j::2],
                                                            identity=ident_b[:]),
                             reads=[B_hid, B_c2], writes=[B_pb2], sig=(j == 1), nowaw=(j > 0))
                    evac(hTt[:, :, :], pb2[:, 0:256].rearrange("p (a b) -> p a b", a=2), [B_pb2], [B_hTt], eng="act")
                    for hf in range(2):
                        ps2, B_ps2 = PF()
                        mmgroup(ps2[:, :], B_ps2, [(hTt[:, j, :], wdn[i][:, j * 1024 + hf * 512:j * 1024 + (hf + 1) * 512]) for j in range(2)],
                                [B_hTt, B_wdn[i]])
                        evac(yb[b % 2][:, s_, hf * 512:(hf + 1) * 512], ps2[:, :], [B_ps2], [B_yb[b % 2]])
                T.dma("sp", lambda e, b=b: e.dma_start(
                    out=Ys_d[b * RB:(b + 1) * RB, :].rearrange("(s p) n -> p s n", p=128), in_=yb[b % 2][:]),
                    reads=[B_yb[b % 2]], writes=[B_Ys], semb=B_yb[b % 2], nowaw=True)
            for en in ("pe", "act", "dve", "pool", "sp"):
                T.finish(en, [B_Ys, B_xT, B_sgl, B_hid, B_hTt] + B_wgu + B_wu + B_wdn + B_xb + B_yb)

        with contextlib.ExitStack() as eC:
            xc = [sb(eC, "xc%d" % i, [128, D], F32) for i in range(2)]
            yg = [sb(eC, "yg%d" % i, [128, 8, D], BF16) for i in range(2)]
            acc = sb(eC, "acc", [128, D], F32)
            jc = sb(eC, "jc", [128, D], BF16)
            oc = [sb(eC, "oc%d" % i, [128, D], F32) for i in range(2)]
            sc = sb(eC, "sc", [128, 4], F32)
            fgrow = sb(eC, "fgrow", [128, D], F32)
            fgr = sb(eC, "fgr", [1, D], F32)
            B_fgr = T.buf("fgr")
            T.dma("sp", lambda e: e.dma_start(out=fgr[:], in_=fng_d[:, :]), writes=[B_fgr])
            bcast_row(fgr[0:1, :], fgrow[:, :], B_fgr, B_fg, False)
            B_xc = [T.buf("xc0"), T.buf("xc1")]
            B_yg = [T.buf("yg0"), T.buf("yg1")]
            B_oc = [T.buf("oc0"), T.buf("oc1")]
            B_acc, B_jc, B_sc, B_out = T.buf("acc"), T.buf("jc"), T.buf("sc"), T.buf("out")

            def load_c(gb):
                i = gb % 2
                T.dma("sp", lambda e: e.dma_start(out=xc[i][:], in_=X1_d[gb * 128:(gb + 1) * 128, :]), reads=[B_X1], writes=[B_xc[i]])
                for k in range(8):
                    T.dma("pool", lambda e, k=k: e.indirect_dma_start(
                        out=yg[i][:, k, :], out_offset=None, in_=Ys_d[:, :],
                        in_offset=bass.IndirectOffsetOnAxis(ap=dest[:, gb, k:k + 1], axis=0), oob_is_err=False),
                        reads=[B_Ys, B_dest], writes=[B_yg[i]], nowaw=(k > 0))

            load_c(0)
            for gb in range(NB):
                if gb + 1 < NB:
                    load_c(gb + 1)
                i = gb % 2
                RC = dict(reads=[B_yg[i], B_w8, B_acc, B_xc[i], B_rows, B_sc, B_fg], writes=[B_acc, B_sc])
                if stage >= 3:
                    T.op("dve", lambda e: e.tensor_scalar(out=acc[:], in0=yg[i][:, 0, :], scalar1=w8[:, gb, 0:1], scalar2=None, op0=ALU.mult), **RC)
                    for k in range(1, 8):
                        T.op("dve", lambda e, k=k: e.scalar_tensor_tensor(out=acc[:], in0=yg[i][:, k, :], scalar=w8[:, gb, k:k + 1], in1=acc[:],
                                                                         op0=ALU.mult, op1=ALU.add), **RC)
                    T.op("dve", lambda e: e.tensor_tensor(out=acc[:], in0=acc[:], in1=rows[:, 4, :], op=ALU.mult), **RC)
                    T.op("dve", lambda e: e.tensor_tensor(out=acc[:], in0=acc[:], in1=xc[i][:], op=ALU.add), **RC)
                else:
                    T.op("dve", lambda e: e.tensor_copy(out=acc[:], in_=xc[i][:]), **RC)
                T.op("act", lambda e: e.activation(out=jc[:], in_=acc[:], func=AF.Square, accum_out=sc[:, 0:1]),
                     reads=[B_acc], writes=[B_jc, B_sc])
                T.op("dve", lambda e: e.tensor_scalar(out=sc[:, 1:2], in0=sc[:, 0:1], scalar1=1.0 / D, scalar2=EPS, op0=ALU.mult, op1=ALU.add), fence=True, **RC)
                T.op("act", lambda e: e.activation(out=sc[:, 3:4], in_=sc[:, 1:2], func=AF.Sqrt), reads=[B_sc], writes=[B_sc])
                T.op("dve", lambda e: e.reciprocal(out=sc[:, 2:3], in_=sc[:, 3:4]), fence=True, **RC)
                T.op("dve", lambda e: e.scalar_tensor_tensor(out=oc[i][:], in0=acc[:], scalar=sc[:, 2:3], in1=fgrow[:], op0=ALU.mult, op1=ALU.mult),
                     reads=[B_acc, B_sc, B_fg], writes=[B_oc[i]])
                T.dma("sp", lambda e: e.dma_start(out=out_d[gb * 128:(gb + 1) * 128, :], in_=oc[i][:]),
                      reads=[B_oc[i]], writes=[B_out], semb=B_oc[i], nowaw=True)
            for en in ("sp", "pe", "act", "dve", "pool"):
                T.finish(en, [B_out, B_acc, B_jc, B_sc, B_fg, B_fgr] + B_yg + B_xc + B_oc)
    return nc


def _t5_buckets(dist):
    n = np.maximum(dist, 0)
    max_exact = 16
    large = max_exact + (np.log(np.maximum(n, 1) / max_exact) / np.log(128 / max_exact) * (32 - max_exact)).astype(np.int32)
    large = np.minimum(large, 31)
    return np.where(n < max_exact, n, large).astype(np.int32)


def _host_consts():
    c = np.zeros((128, 640), np.float32)
    c[:, 0:128] = np.eye(128, dtype=np.float32)
    c[:, 128:256] = np.triu(np.ones((128, 128), np.float32), 1)
    c[:, 256] = np.arange(128, dtype=np.float32)
    c[:, 257:513] = (np.arange(256, dtype=np.float32) + 1.0) * 4096.0
    return c


def _prep_shared(inp):
    f = lambda a: np.ascontiguousarray(np.asarray(a, dtype=np.float32))
    w_in = f(inp["w_in"][0])
    qcols = []
    for c in range(4):
        qcols += list(range(c * 64, (c + 1) * 64)) + list(range((4 + c) * 64, (5 + c) * 64))
    perm = np.array(qcols + list(range(512, 1792)))
    w_in_p = np.ascontiguousarray(w_in[:, perm])
    w_out = f(inp["w_out"][0])
    operm = np.array(qcols + list(range(512, 1024)))
    w_out_p = np.ascontiguousarray(w_out[operm, :])
    hd = np.array([[c if p < 64 else 4 + c for c in range(4)] for p in range(128)])
    sinkfm = f(inp["attn_sinks"][0])[hd]
    ang = f(inp["attn_norm_g"][0])
    angfm = np.ascontiguousarray(ang[np.array(qcols)].reshape(4, 128).T)
    fm4 = lambda v: np.ascontiguousarray(f(v).reshape(4, 128).T)
    convw = np.ascontiguousarray(f(inp["conv_w"][0]).T.reshape(4, 128, 31).transpose(1, 0, 2).reshape(128, 124))
    rel = f(inp["rel_bias"])
    s_ = np.arange(128)[:, None]
    q_ = np.arange(128)[None, :]
    biasg = np.zeros((2, 2, 128, 512), np.float32)
    maskc = np.zeros((2, 128, 512), np.float32)
    for j in range(2):
        dist = q_ + (128 if j == 0 else 0) - s_
        bk = _t5_buckets(dist)
        valid = (dist >= 0) & (dist < 128)
        for g in range(2):
            for i in range(4):
                biasg[j, g, :, i * 128:(i + 1) * 128] = rel[bk, g * 4 + i]
        maskc[j] = np.tile(np.where(valid, 0.0, -80000.0).astype(np.float32), (1, 4))
    sh = dict(
        w_ada=f(inp["w_ada"][0]), b_ada=f(inp["b_ada"][0]).reshape(1, -1), w_in=w_in_p, sinkfm=np.ascontiguousarray(sinkfm),
        biasg=biasg, maskc=maskc, angfm=angfm, convw=convw, convb=fm4(inp["conv_b"][0]), lng=fm4(inp["conv_ln_g"][0]),
        lnb=fm4(inp["conv_ln_b"][0]), w_out=w_out_p, w_router=f(inp["w_router"][0]), router_bias=f(inp["router_bias"][0]).reshape(1, -1),
        w_exp_gate=f(inp["w_exp_gate"][0]).reshape(E * 128, 2048), w_exp_up=f(inp["w_exp_up"][0]).reshape(E * 128, 2048),
        w_exp_down=f(inp["w_exp_down"][0]).reshape(E * 128, 2048), w_sh_gate=f(inp["w_sh_gate"][0]), w_sh_up=f(inp["w_sh_up"][0]),
        w_sh_down=f(inp["w_sh_down"][0]), final_norm_g=f(inp["final_norm_g"]).reshape(1, -1), consts=_host_consts(),
    )
    return sh


def kernel(x, c, w_ada, b_ada, w_in, attn_sinks, rel_bias, attn_norm_g, conv_w, conv_b, conv_ln_g, conv_ln_b, w_out,
           w_router, router_bias, w_exp_gate, w_exp_up, w_exp_down, w_sh_gate, w_sh_up, w_sh_down, final_norm_g):
    inp = dict(w_ada=w_ada, b_ada=b_ada, w_in=w_in, attn_sinks=attn_sinks, rel_bias=rel_bias, attn_norm_g=attn_norm_g,
               conv_w=conv_w, conv_b=conv_b, conv_ln_g=conv_ln_g, conv_ln_b=conv_ln_b, w_out=w_out, w_router=w_router,
               router_bias=router_bias, w_exp_gate=w_exp_gate, w_exp_up=w_exp_up, w_exp_down=w_exp_down,
               w_sh_gate=w_sh_gate, w_sh_up=w_sh_up, w_sh_down=w_sh_down, final_norm_g=final_norm_g)
    stage = int(os.environ.get("MK_STAGE", "99"))
    cores = [int(v) for v in os.environ.get("MK_CORES", "0,1,2,3,4,5,6,7").split(",")]
    sh = _prep_shared(inp)
    x = np.asarray(x, dtype=np.float32)
    c = np.asarray(c, dtype=np.float32)
    in_maps = []
    if os.environ.get("MK_SMALLW"):
        n_ = int(os.environ["MK_SMALLW"]) * 128
        for k_ in ("w_exp_gate", "w_exp_up", "w_exp_down"):
            sh[k_] = np.ascontiguousarray(sh[k_][:n_])
    for b in cores:
        m = dict(sh)
        m["x"] = np.ascontiguousarray(x[b])
        m["cfm"] = np.ascontiguousarray(c[b].reshape(8, 128).T)
        in_maps.append(m)
    nc = build(stage)
    group = int(os.environ.get("MK_GROUP", "1"))
    outs = []
    for g0 in range(0, len(in_maps), group):
        sub = in_maps[g0:g0 + group]
        res = run_bass_kernel_spmd(nc, sub, core_ids=list(range(len(sub))))
        outs += [np.asarray(r["out"], dtype=np.float32) for r in res.results]
    if len(cores) < 8:
        return np.stack(outs, axis=0)
    return np.stack(outs, axis=0).astype(np.float32)
```
